# Optimizing a Trainium2 kernel written in Bass

```python
import math
import jax
import jax.numpy as jnp
from jax import lax
import numpy as np

D_MODEL = 2048
BATCH = 2
SEQ = 4096
DEPTH = 2

GRID_W = 64
WIN_ROWS = 8
WIN_COLS = 16
Q_COLS = 16
BAND_COLS = Q_COLS + WIN_COLS
NA_HEADS = 8
NA_HEAD_DIM = 128
NA_WIDTH = NA_HEADS * NA_HEAD_DIM
CONV_CH = D_MODEL // 2
CONV_WIDTH = 3
MLA_HEADS = 8
Q_LORA = 512
KV_LORA = 512
QK_NOPE = 128
QK_ROPE = 64
V_DIM = 128
ROPE_THETA = 10000.0
Q_BLOCK = 128
SG_CH = D_MODEL // 2
SG_GROUPS = 8
CHUNK = 128
N_GROUPS = 8
EXPERTS_PER_GROUP = 8
N_EXPERTS = N_GROUPS * EXPERTS_PER_GROUP
TOP_K = 2
D_EXPERT = 768
MOE_BLOCK = 128
EPS = 1e-6
NEG_INF = -1e30
AB_IN = 3 * NA_WIDTH + 3 * CONV_CH
AB_OUT = NA_WIDTH + CONV_CH
CD_IN = Q_LORA + KV_LORA + QK_ROPE + 2 * SG_CH
CD_OUT = MLA_HEADS * V_DIM + SG_CH

kernel_name = 'hybrid_natten_conv_mla_sgu_hmoe_encoder'


def rms_norm(x, g):
    xf = x.astype(jnp.float32)
    y = xf * lax.rsqrt(jnp.mean(xf * xf, axis=-1, keepdims=True) + EPS)
    return (y * g.astype(jnp.float32)).astype(x.dtype)


def neighbourhood_attention(q, k, v, rpb):
    bsz, seq, heads, dh = q.shape
    rows = seq // GRID_W
    kr = min(WIN_ROWS, rows)
    n_cb = GRID_W // Q_COLS
    n_keys = kr * BAND_COLS
    r = np.arange(rows)
    key_rows = np.clip(r - kr // 2, 0, rows - kr)[:, None] + np.arange(kr)[None, :]
    band_start = np.clip(np.arange(n_cb) * Q_COLS - WIN_COLS // 2, 0, GRID_W - BAND_COLS)
    key_cols = band_start[:, None] + np.arange(BAND_COLS)[None, :]
    q_cols = np.arange(n_cb)[:, None] * Q_COLS + np.arange(Q_COLS)[None, :]
    col_start = np.clip(q_cols - WIN_COLS // 2, 0, GRID_W - WIN_COLS)
    in_win = (key_cols[:, None, :] >= col_start[:, :, None]) & (key_cols[:, None, :] < col_start[:, :, None] + WIN_COLS)
    valid = np.broadcast_to(in_win[:, :, None, :], (n_cb, Q_COLS, kr, BAND_COLS)).reshape(n_cb, Q_COLS, n_keys)
    dr_idx = (key_rows - r[:, None] + WIN_ROWS - 1).astype(np.int32)
    dc_idx = np.clip(key_cols[:, None, :] - q_cols[:, :, None] + WIN_COLS - 1, 0, 2 * WIN_COLS - 2).astype(np.int32)
    bias = rpb[:, dr_idx[:, None, None, :, None], dc_idx[None, :, :, None, :]]
    bias = bias.reshape(heads, rows, n_cb, Q_COLS, n_keys)
    key_tok = (key_rows[:, None, :, None] * GRID_W + key_cols[None, :, None, :]).reshape(-1).astype(np.int32)
    qg = q.transpose(0, 2, 1, 3).reshape(bsz, heads, rows, n_cb, Q_COLS, dh)
    kg = jnp.take(k.transpose(0, 2, 1, 3), key_tok, axis=2).reshape(bsz, heads, rows, n_cb, n_keys, dh)
    vg = jnp.take(v.transpose(0, 2, 1, 3), key_tok, axis=2).reshape(bsz, heads, rows, n_cb, n_keys, dh)
    s = jnp.einsum('bhrjqd,bhrjkd->bhrjqk', qg, kg).astype(jnp.float32) * (dh ** -0.5) + bias.astype(jnp.float32)
    s = jnp.where(valid, s, NEG_INF)
    p = jax.nn.softmax(s, axis=-1).astype(v.dtype)
    o = jnp.einsum('bhrjqk,bhrjkd->bhrjqd', p, vg)
    return o.reshape(bsz, heads, seq, dh).transpose(0, 2, 1, 3).reshape(bsz, seq, heads * dh)


def short_conv(z, w, b):
    seq = z.shape[1]
    pad = CONV_WIDTH // 2
    zp = jnp.pad(z, ((0, 0), (pad, CONV_WIDTH - 1 - pad), (0, 0)))
    y = b
    for tap in range(CONV_WIDTH):
        y = y + zp[:, tap:tap + seq] * w[tap]
    return y


def rope_angles(seq):
    pos = jnp.arange(seq, dtype=jnp.int32)
    row = (pos // GRID_W).astype(jnp.float32)
    col = (pos % GRID_W).astype(jnp.float32)
    half = QK_ROPE // 2
    inv = ROPE_THETA ** (-jnp.arange(0, half, 2, dtype=jnp.float32) / half)
    return row[:, None] * inv, col[:, None] * inv


def rotate(x, ang):
    m = x.shape[-1] // 2
    x1, x2 = x[..., :m], x[..., m:]
    c = jnp.cos(ang).astype(x.dtype)
    s = jnp.sin(ang).astype(x.dtype)
    return jnp.concatenate([x1 * c - x2 * s, x1 * s + x2 * c], axis=-1)


def axial_rope(x, ang_row, ang_col):
    m = x.shape[-1] // 2
    return jnp.concatenate([rotate(x[..., :m], ang_row), rotate(x[..., m:], ang_col)], axis=-1)


def mla(c_q, c_kv, k_r, q_norm, kv_norm, w_uq, w_ukv, ang_row, ang_col):
    bsz, seq, _ = c_q.shape
    q = (rms_norm(c_q, q_norm) @ w_uq).reshape(bsz, seq, MLA_HEADS, QK_NOPE + QK_ROPE)
    q_nope = q[..., :QK_NOPE]
    q_rope = axial_rope(q[..., QK_NOPE:], ang_row[:, None], ang_col[:, None])
    kv = (rms_norm(c_kv, kv_norm) @ w_ukv).reshape(bsz, seq, MLA_HEADS, QK_NOPE + V_DIM)
    k_nope = kv[..., :QK_NOPE].transpose(0, 2, 1, 3)
    vh = kv[..., QK_NOPE:].transpose(0, 2, 1, 3)
    k_rope = axial_rope(k_r, ang_row, ang_col)
    scale = (QK_NOPE + QK_ROPE) ** -0.5
    nb = seq // Q_BLOCK
    qn_b = q_nope.reshape(bsz, nb, Q_BLOCK, MLA_HEADS, QK_NOPE).transpose(1, 0, 3, 2, 4)
    qr_b = q_rope.reshape(bsz, nb, Q_BLOCK, MLA_HEADS, QK_ROPE).transpose(1, 0, 3, 2, 4)

    def attend(blk):
        qn, qr = blk
        s = jnp.einsum('bhqd,bhkd->bhqk', qn, k_nope) + jnp.einsum('bhqr,bkr->bhqk', qr, k_rope)
        p = jax.nn.softmax(s.astype(jnp.float32) * scale, axis=-1).astype(vh.dtype)
        return jnp.einsum('bhqk,bhkd->bhqd', p, vh)

    o = lax.map(attend, (qn_b, qr_b))
    return o.transpose(1, 0, 3, 2, 4).reshape(bsz, seq, MLA_HEADS * V_DIM)


def spatial_gating(u, v, g_norm, w_s, b_s):
    u = jax.nn.gelu(u)
    v = rms_norm(jax.nn.gelu(v), g_norm)
    bsz, seq, ch = v.shape
    vr = v.reshape(bsz, seq // CHUNK, CHUNK, SG_GROUPS, ch // SG_GROUPS)
    mixed = jnp.einsum('gpq,bnqgc->bnpgc', w_s, vr) + b_s.T[None, None, :, :, None]
    return u * mixed.reshape(bsz, seq, ch)


def hier_moe(x, wg, bg, we, be, w1, w3, w2):
    bsz, seq, dm = x.shape
    xf = x.reshape(-1, dm)
    n_tok = xf.shape[0]
    g_logits = (xf @ wg).astype(jnp.float32) + bg.astype(jnp.float32)
    g_idx = jnp.argmax(g_logits, axis=-1)
    g_prob = jnp.take_along_axis(jax.nn.softmax(g_logits, axis=-1), g_idx[:, None], axis=-1)[:, 0]
    e_logits = ((xf @ we).astype(jnp.float32) + be.astype(jnp.float32)).reshape(n_tok, N_GROUPS, EXPERTS_PER_GROUP)
    e_in = jnp.take_along_axis(e_logits, g_idx[:, None, None], axis=1)[:, 0]
    top_v, top_i = lax.top_k(e_in, TOP_K)
    gate = g_prob[:, None] * jax.nn.softmax(top_v, axis=-1)
    eid = (g_idx[:, None] * EXPERTS_PER_GROUP + top_i).reshape(-1).astype(jnp.int32)
    tok = jnp.repeat(jnp.arange(n_tok, dtype=jnp.int32), TOP_K)
    gate_flat = gate.reshape(-1)
    order = jnp.argsort(eid)
    e_s, tok_s, w_s = eid[order], tok[order], gate_flat[order]
    counts = jnp.zeros((N_EXPERTS,), jnp.int32).at[eid].add(1)
    starts = jnp.cumsum(counts) - counts
    padded = (counts + MOE_BLOCK - 1) // MOE_BLOCK * MOE_BLOCK
    pad_ends = jnp.cumsum(padded)
    pad_starts = pad_ends - padded
    n_assign = n_tok * TOP_K
    dest = pad_starts[e_s] + (jnp.arange(n_assign, dtype=jnp.int32) - starts[e_s])
    n_blocks = (n_assign + MOE_BLOCK - 1) // MOE_BLOCK + N_EXPERTS
    n_rows = n_blocks * MOE_BLOCK
    row_tok = jnp.full((n_rows,), n_tok, jnp.int32).at[dest].set(tok_s)
    row_w = jnp.zeros((n_rows,), x.dtype).at[dest].set(w_s.astype(x.dtype))
    blk_expert = jnp.searchsorted(pad_ends, jnp.arange(n_blocks, dtype=jnp.int32) * MOE_BLOCK, side='right')
    blk_expert = jnp.minimum(blk_expert, N_EXPERTS - 1).astype(jnp.int32)
    x_pad = jnp.concatenate([xf, jnp.zeros((1, dm), xf.dtype)], axis=0)
    xb = x_pad[row_tok].reshape(n_blocks, MOE_BLOCK, dm)

    def expert_block(args):
        xblk, e = args
        h = jax.nn.silu(xblk @ w1[e]) * (xblk @ w3[e])
        return h @ w2[e]

    yb = lax.map(expert_block, (xb, blk_expert)).reshape(n_rows, dm)
    out = jnp.zeros((n_tok + 1, dm), x.dtype).at[row_tok].add(yb * row_w[:, None])[:n_tok]
    return out.reshape(bsz, seq, dm)


def setup_inputs(seed: int = 0) -> dict:
    key = jax.random.key(seed)
    ks = jax.random.split(key, 26)
    ne = (DEPTH + 1) // 2
    no = DEPTH // 2
    f32 = jnp.float32

    def nrm(k, shape, scale):
        return jax.random.normal(k, shape, f32) * scale

    def gain(k, shape):
        return 1.0 + 0.05 * jax.random.normal(k, shape, f32)

    return {
        'x': nrm(ks[0], (BATCH, SEQ, D_MODEL), 1.0),
        'norm_mix': gain(ks[1], (DEPTH, D_MODEL)),
        'norm_ffn': gain(ks[2], (DEPTH, D_MODEL)),
        'norm_final': gain(ks[3], (D_MODEL,)),
        'w_in_ab': nrm(ks[4], (ne, D_MODEL, AB_IN), D_MODEL ** -0.5),
        'na_rpb': nrm(ks[5], (ne, NA_HEADS, 2 * WIN_ROWS - 1, 2 * WIN_COLS - 1), 0.1),
        'conv_w': nrm(ks[6], (ne, CONV_WIDTH, CONV_CH), CONV_WIDTH ** -0.5),
        'conv_b': nrm(ks[7], (ne, CONV_CH), 0.02),
        'w_out_ab': nrm(ks[8], (ne, AB_OUT, D_MODEL), AB_OUT ** -0.5),
        'w_in_cd': nrm(ks[9], (no, D_MODEL, CD_IN), D_MODEL ** -0.5),
        'q_norm': gain(ks[10], (no, Q_LORA)),
        'kv_norm': gain(ks[11], (no, KV_LORA)),
        'w_uq': nrm(ks[12], (no, Q_LORA, MLA_HEADS * (QK_NOPE + QK_ROPE)), Q_LORA ** -0.5),
        'w_ukv': nrm(ks[13], (no, KV_LORA, MLA_HEADS * (QK_NOPE + V_DIM)), KV_LORA ** -0.5),
        'sg_norm': gain(ks[14], (no, SG_CH)),
        'sg_w': nrm(ks[15], (no, SG_GROUPS, CHUNK, CHUNK), CHUNK ** -0.5),
        'sg_b': nrm(ks[16], (no, SG_GROUPS, CHUNK), 0.02),
        'w_out_cd': nrm(ks[17], (no, CD_OUT, D_MODEL), CD_OUT ** -0.5),
        'router_group_w': nrm(ks[18], (DEPTH, D_MODEL, N_GROUPS), D_MODEL ** -0.5),
        'router_group_b': nrm(ks[19], (DEPTH, N_GROUPS), 0.01),
        'router_expert_w': nrm(ks[20], (DEPTH, D_MODEL, N_EXPERTS), D_MODEL ** -0.5),
        'router_expert_b': nrm(ks[21], (DEPTH, N_EXPERTS), 0.01),
        'w1': nrm(ks[22], (DEPTH, N_EXPERTS, D_MODEL, D_EXPERT), D_MODEL ** -0.5),
        'w3': nrm(ks[23], (DEPTH, N_EXPERTS, D_MODEL, D_EXPERT), D_MODEL ** -0.5),
        'w2': nrm(ks[24], (DEPTH, N_EXPERTS, D_EXPERT, D_MODEL), D_EXPERT ** -0.5),
    }


def reference(x, norm_mix, norm_ffn, norm_final, w_in_ab, na_rpb, conv_w, conv_b, w_out_ab,
              w_in_cd, q_norm, kv_norm, w_uq, w_ukv, sg_norm, sg_w, sg_b, w_out_cd,
              router_group_w, router_group_b, router_expert_w, router_expert_b, w1, w3, w2):
    bsz, seq, _ = x.shape
    ang_row, ang_col = rope_angles(seq)
    ab_split = [NA_WIDTH, 2 * NA_WIDTH, 3 * NA_WIDTH, 3 * NA_WIDTH + CONV_CH, 3 * NA_WIDTH + 2 * CONV_CH]
    cd_split = [Q_LORA, Q_LORA + KV_LORA, Q_LORA + KV_LORA + QK_ROPE, Q_LORA + KV_LORA + QK_ROPE + SG_CH]
    for layer in range(DEPTH):
        i = layer // 2
        h = rms_norm(x, norm_mix[layer])
        if layer % 2 == 0:
            p = h @ w_in_ab[i]
            q, k, v, gate_b, gate_c, hc = jnp.split(p, ab_split, axis=-1)
            heads = lambda t: t.reshape(bsz, seq, NA_HEADS, NA_HEAD_DIM)
            a_out = neighbourhood_attention(heads(q), heads(k), heads(v), na_rpb[i])
            b_out = gate_b * short_conv(gate_c * hc, conv_w[i], conv_b[i])
            x = x + jnp.concatenate([a_out, b_out], axis=-1) @ w_out_ab[i]
        else:
            p = h @ w_in_cd[i]
            c_q, c_kv, k_r, u, vv = jnp.split(p, cd_split, axis=-1)
            c_out = mla(c_q, c_kv, k_r, q_norm[i], kv_norm[i], w_uq[i], w_ukv[i], ang_row, ang_col)
            d_out = spatial_gating(u, vv, sg_norm[i], sg_w[i], sg_b[i])
            x = x + jnp.concatenate([c_out, d_out], axis=-1) @ w_out_cd[i]
        x = x + hier_moe(rms_norm(x, norm_ffn[layer]), router_group_w[layer], router_group_b[layer],
                         router_expert_w[layer], router_expert_b[layer], w1[layer], w3[layer], w2[layer])
    return rms_norm(x, norm_final)
```

```python
import numpy as np
import ml_dtypes
import concourse.bass as bass
import concourse.mybir as mybir
from concourse.bass_utils import run_bass_kernel_spmd

F32 = mybir.dt.float32
BF16 = mybir.dt.bfloat16
AF = mybir.ActivationFunctionType
ALU = mybir.AluOpType
AX = mybir.AxisListType
NPBF = ml_dtypes.bfloat16

EPS = 1e-6
NEG = -30000.0
ENGS = ("pe", "dve", "act", "pool", "sp")


class Tok:
    __slots__ = ("kind", "eng", "sem", "value")

    def __init__(self, kind, eng=None, sem=None, value=None):
        self.kind, self.eng, self.sem, self.value = kind, eng, sem, value


class Buf:
    __slots__ = ("name", "w", "readers", "const", "excl")

    def __init__(self, name, const=False, excl=False):
        self.name, self.w, self.readers, self.const, self.excl = name, None, [], const, excl


class Prog:
    def __init__(self, nc, block, sems):
        self.nc, self.block = nc, block
        self.q = {e: [] for e in ENGS}
        self.cnt = {e: 0 for e in ENGS}
        self.esem = {e: sems.pop() for e in ENGS}
        self.free_sems = sems
        self.waited = {e: {} for e in ENGS}
        self.last = {e: None for e in ENGS}
        self.dma_last = {}
        self.pending = {e: [] for e in ENGS}
        self.n_ins = 0

    def new_sem(self):
        return self.free_sems.pop()

    def _deps(self, eng, reads, writes, issue=None):
        issue = issue or eng
        deps = list(self.pending[issue])
        self.pending[issue] = []
        for b in reads:
            if b.w is not None:
                deps.append(b.w)
            if b.excl:
                for r in b.readers:
                    if r.kind == "dma" or r.eng != eng:
                        deps.append(r)
        for b in writes:
            if b.w is not None and (b.w.kind == "dma" or b.w.eng != eng or eng != "pe"):
                deps.append(b.w)
            for r in b.readers:
                if r.kind == "dma" or r.eng != eng or eng != "pe":
                    deps.append(r)
        return deps

    def op(self, eng, fn, reads=(), writes=()):
        deps = self._deps(eng, reads, writes)
        self.cnt[eng] += 1
        tok = Tok("eng", eng=eng, sem=self.esem[eng], value=self.cnt[eng])
        self.q[eng].append((fn, deps, tok))
        self.last[eng] = tok
        for b in reads:
            if not b.const:
                b.readers.append(tok)
        for b in writes:
            b.w, b.readers = tok, []
        return tok

    def dma(self, eng, fn, dst, srcs, sem_state):
        deps = self._deps("dma", srcs, [dst], issue=eng)
        sem_state[1] += 16
        tok = Tok("dma", sem=sem_state[0], value=sem_state[1])
        self.q[eng].append((fn, deps, tok))
        self.dma_last[id(sem_state[0])] = tok
        for b in srcs:
            if not b.const:
                b.readers.append(tok)
        dst.w, dst.readers = tok, []
        return tok

    def barrier(self):
        toks = [t for t in self.last.values() if t is not None] + list(self.dma_last.values())
        for e in ENGS:
            self.pending[e] = list(toks)

    def flush(self):
        eng_fn = {"pe": self.block.tensor, "dve": self.block.vector, "act": self.block.scalar,
                  "pool": self.block.gpsimd, "sp": self.block.sync}
        for e in ENGS:
            recs = self.q[e]
            if not recs:
                continue
            self.q[e] = []
            waited = self.waited[e]

            def body(eng, recs=recs, waited=waited):
                for fn, deps, tok in recs:
                    for d in deps:
                        k = id(d.sem)
                        if waited.get(k, 0) < d.value:
                            eng.wait_ge(d.sem, d.value)
                            waited[k] = d.value
                    ins = fn(eng)
                    ins.then_inc(tok.sem, 16 if tok.kind == "dma" else 1)
                    self.n_ins += 1

            eng_fn[e](body)

    def final_wait(self, eng):
        toks = [t for t in self.last.values() if t is not None] + list(self.dma_last.values())
        recs_waited = self.waited[eng]

        def body(en):
            for d in toks:
                k = id(d.sem)
                if recs_waited.get(k, 0) < d.value:
                    en.wait_ge(d.sem, d.value)
                    recs_waited[k] = d.value

        {"pe": self.block.tensor, "dve": self.block.vector, "act": self.block.scalar,
         "pool": self.block.gpsimd, "sp": self.block.sync}[eng](body)


def _na_tables(rpb):
    kc = np.arange(64)[:, None]
    qc = np.arange(64)[None, :]
    col_start = np.clip(qc - 8, 0, 48)
    colvalid = (kc >= col_start) & (kc < col_start + 16)
    dc = np.clip(kc - qc + 15, 0, 30)
    A = np.full((128, 8, 8, 64), NEG, np.float32)
    for t in range(15):
        m, par = t // 2, t % 2
        vals = rpb[:, t, :][:, dc]
        vals = np.where(colvalid[None], vals, np.float32(NEG)).astype(np.float32)
        A[par * 64:(par + 1) * 64, :, m, :] = vals.transpose(1, 0, 2)
    return A.reshape(128, 4096)


def _row_mask(j):
    R = np.full((16, 16, 64), NEG, np.float32)
    for i in range(16):
        g = 16 * j + i
        k0 = min(max(g - 4, 0), 56)
        for t in range(16):
            kr = g - 7 + t
            if k0 <= kr <= k0 + 7:
                R[t, i, :] = 0.0
    return R.reshape(16, 1024).astype(NPBF)


def _rope_tables(j):
    half = 32
    inv = (np.float32(10000.0) ** (-np.arange(0, half, 2, dtype=np.float32) / np.float32(half))).astype(np.float32)
    pos = np.arange(1024) + 1024 * j
    row = (pos // 64).astype(np.float32)
    col = (pos % 64).astype(np.float32)
    ar = row[:, None] * inv[None, :]
    ac = col[:, None] * inv[None, :]
    C = np.zeros((64, 1024), np.float32)
    S = np.zeros((64, 1024), np.float32)
    for d in range(64):
        ang = ar if d < 32 else ac
        f = (d % 32) % 16
        C[d] = np.cos(ang[:, f])
        s = np.sin(ang[:, f])
        S[d] = -s if (d % 32) < 16 else s
    return np.stack([C, S]).astype(np.float32)


def _consts():
    ident = np.eye(128, dtype=np.float32)
    ustrict = np.triu(np.ones((128, 128), np.float32), 1)
    iota = np.tile(np.arange(128, dtype=np.float32)[None, :], (128, 1))
    em = np.zeros((16, 8, 128), np.float32)
    for m in range(8):
        for p in range(128):
            em[2 * m + p // 64, m, p] = 1.0
    prot = np.zeros((64, 64), np.float32)
    for d in range(64):
        partner = d + 16 if (d % 32) < 16 else d - 16
        prot[partner, d] = 1.0
    return dict(ident_bf=ident.astype(NPBF), ident_f=ident, ones_bf=np.ones((128, 128), NPBF),
                ustrict=ustrict.astype(NPBF), iota_f=iota, em=em.reshape(16, 1024).astype(NPBF),
                prot=prot.astype(NPBF))


BASE = 16512
LIMIT = 229344


def build_program(upto=99, debug=False, NEXP=64, NL=2, skip_l0=False, skip_l1=False):
    nc = bass.Bass("TRN2", target_bir_lowering=False)

    declared = []

    def din(name, shape, dt=F32):
        declared.append(name)
        return nc.dram_tensor(name, list(shape), dt, kind="ExternalInput").ap()

    xe = din("xe", [1536, 2048])
    gvec = din("gvec", [5, 128, 2048])
    if not skip_l0:
        w_in_ab = din("w_in_ab", [2048, 6144])
        w_out_ab = din("w_out_ab", [2048, 2048])
        atab_d = din("atab", [128, 4096])
        em_d = din("em", [16, 1024], BF16)
        rq_d = din("rq", [16, 1024], BF16)
        cwb_d = din("cwb", [128, 32])
    ident_bf_d = din("ident_bf", [128, 128], BF16)
    ident_f_d = din("ident_f", [128, 128])
    ones_bf_d = din("ones_bf", [128, 128], BF16)
    ustrict_d = din("ustrict", [128, 128], BF16)
    iota_f_d = din("iota_f", [128, 128])
    w1_d, w3_d, w2_d = {}, {}, {}
    if upto >= 4:
        wr_d = din("wr", [2, 2048, 72])
        br_d = din("br", [2, 128, 72])
        for l in range(2):
            if (l == 0 and not skip_l0) or (l == 1 and upto >= 7):
                w1_d[l] = din(f"w1_{l}", [NEXP, 2048, 768])
                w3_d[l] = din(f"w3_{l}", [NEXP, 2048, 768])
                w2_d[l] = din(f"w2_{l}", [NEXP, 768, 2048])
    if upto >= 5 and not skip_l1:
        w_in_cd = din("w_in_cd", [2048, 3136])
        qkn_d = din("qkn", [2, 128, 512])
        w_uq = din("w_uq", [512, 1536])
        w_ukv = din("w_ukv", [512, 2048])
        sgn_d = din("sgn", [128, 1024])
        wsT_d = din("wsT", [128, 1024])
        bsT_d = din("bsT", [128, 1024])
        w_out_cd = din("w_out_cd", [2048, 2048])
        rope_d = din("ropecs", [2, 64, 1024])
        prot_d = din("prot", [64, 64], BF16)
        bm_d = din("bm", [128, 2])
    y_d = nc.dram_tensor("y", [1024, 2048], F32, kind="ExternalOutput").ap()
    dbg_d = nc.dram_tensor("dbg", [1024, 2048], F32, kind="ExternalOutput").ap() if debug else None

    def sbt(name, shape, dt, off):
        nbytes = int(np.prod(shape[1:])) * (4 if dt == F32 else 2)
        assert off % 32 == 0 and BASE + off + nbytes <= LIMIT, (name, off, nbytes)
        return nc.alloc_sbuf_tensor_at(name, list(shape), dt, offset=BASE + off)

    from contextlib import ExitStack
    with ExitStack() as es:
        sems = [es.enter_context(nc.semaphore(f"s{i}")) for i in range(17)]
        psum = [es.enter_context(nc.psum_tensor(f"ps{i}", [128, 512], F32)) for i in range(8)]
        block = es.enter_context(nc.Block())
        P = Prog(nc, block, sems)
        PB = [Buf(f"psum{i}", excl=True) for i in range(8)]

        def dsem():
            return [P.new_sem(), 0]

        xres = sbt("xres", [128, 8, 2048], F32, 0)
        B_xres = [Buf(f"xres{i}") for i in range(8)]
        CO = 65536
        ident_bf = sbt("ident_bf", [128, 128], BF16, CO)
        ident_f = sbt("ident_f", [128, 128], F32, CO + 256)
        ones_bf = sbt("ones_bf", [128, 128], BF16, CO + 768)
        ustrict = sbt("ustrict", [128, 128], BF16, CO + 1024)
        iota_f = sbt("iota_f", [128, 128], F32, CO + 1280)
        small = sbt("small", [128, 64], F32, CO + 1792)
        B_const = Buf("consts", const=True)
        B_small = [Buf(f"small{i}") for i in range(64)]
        PH = CO + 2048
        PHSZ = LIMIT - BASE - PH

        csem = dsem()
        SP_ = [dsem() for _ in range(11)]

        B_cchain = Buf("cchain")

        def cload(dst_ap, src_ap, buf=None, eng="sp"):
            t = P.dma(eng, lambda e: e.dma_start(out=dst_ap, in_=src_ap), buf or B_const, [B_cchain], csem)
            B_cchain.w, B_cchain.readers = t, []

        cload(ident_bf[:], ident_bf_d)
        cload(ident_f[:], ident_f_d)
        cload(ones_bf[:], ones_bf_d)
        cload(ustrict[:], ustrict_d)
        cload(iota_f[:], iota_f_d)

        def mm(out_ap, pairs, reads, writes):
            def fn(e):
                n = len(pairs)
                ins = None
                for i, (l, r) in enumerate(pairs):
                    ins = e.matmul(out_ap, l, r, start=(i == 0), stop=(i == n - 1))
                return ins
            return P.op("pe", fn, reads, writes)

        def rmsnorm(src_ap, src_bufs, W, g_ap, g_buf, out_ap, out_buf, junk_ap, junk_buf, si):
            ss, rs = small[:, si:si + 1], small[:, si + 1:si + 2]
            Bss, Brs = B_small[si], B_small[si + 1]
            P.op("act", lambda e: e.activation(out=junk_ap, in_=src_ap, func=AF.Square, accum_out=ss),
                 src_bufs, [junk_buf, Bss])
            P.op("dve", lambda e: e.tensor_scalar(out=rs, in0=ss, scalar1=1.0 / W, scalar2=EPS,
                                                  op0=ALU.mult, op1=ALU.add), [Bss], [Brs])
            P.op("act", lambda e: e.activation(out=rs, in_=rs, func=AF.Sqrt), [Brs], [Brs])
            P.op("dve", lambda e: e.reciprocal(out=rs, in_=rs), [Brs], [Brs])
            P.op("dve", lambda e: e.scalar_tensor_tensor(out=out_ap, in0=src_ap, scalar=rs, in1=g_ap,
                                                         op0=ALU.mult, op1=ALU.mult),
                 list(src_bufs) + [Brs, g_buf], [out_buf])

        def transposes_bf(src, src_buf, nchunk, dst_fn, dst_buf, pi):
            for c0 in range(0, nchunk, 8):
                n = min(8, nchunk - c0)
                pt = psum[pi].bitcast(BF16)[:, 0:n * 128].rearrange("p (c t) -> p c t", t=128)

                def fn(e, c0=c0, n=n, pt=pt):
                    ins = None
                    for c in range(n):
                        ins = e.transpose(pt[:, c, :], src[:, (c0 + c) * 128:(c0 + c + 1) * 128], ident_bf[:])
                    return ins
                P.op("pe", fn, [src_buf, B_const], [PB[pi]])
                d = dst_fn(c0, n)
                P.op("act", lambda e, d=d, pt=pt: e.copy(out=d, in_=pt), [PB[pi]], [dst_buf])
                pi = 6 + (pi - 6 + 1) % 2
            return pi

        RUN_L0 = not skip_l0
        hT = sbt("hT", [128, 16, 1664], BF16, 0)
        xn = sbt("xn", [128, 2048], BF16, 53248)
        junk = sbt("junk", [128, 2048], BF16, 57344)
        o = PH
        boutT = sbt("boutT", [128, 8, 1024], BF16, o); o += 16384
        KT = sbt("KT", [128, 8, 1600], BF16, o); o += 25600
        QT = sbt("QT", [128, 8, 1024], BF16, o); o += 16384
        VV = sbt("VV", [128, 24, 1024], BF16, o); o_v = o; o += 49152
        wbuf = [sbt(f"wbuf{i}", [128, 16, 512], BF16, o + i * 16384) for i in range(2)]
        o_w = o; o += 32768
        assert o <= PH + PHSZ, o
        xt = [sbt(f"xt{i}", [128, 2048], F32, o_v + 32768 + i * 8192) for i in range(2)]
        gbc = sbt("gbc", [128, 2048], F32, o_v + 24576)
        cwb = sbt("cwb", [128, 32], F32, CO + 1792 + 128)
        B_hT, B_xn, B_junk, B_gbc = Buf("hT"), Buf("xn"), Buf("junk"), Buf("gbc")
        B_xt = [Buf("xt0"), Buf("xt1")]
        B_bout, B_KT, B_QT, B_VV = Buf("boutT"), Buf("KT"), Buf("QT"), Buf("VV")
        B_wbuf = [Buf("wbuf0"), Buf("wbuf1")]
        wsem = [SP_[0], SP_[1]]
        xsem = [SP_[2], SP_[3]]

        if RUN_L0:
            cload(gbc[:], gvec[0], B_gbc)
            cload(cwb[:], cwb_d)
            P.op("pool", lambda e: e.memset(hT[:, :, 0:64], 0.0), [], [B_hT])
            P.op("pool", lambda e: e.memset(hT[:, :, 1600:1664], 0.0), [], [B_hT])
        pi = 6
        for k in (range(12) if RUN_L0 else ()):
            s = k % 2
            P.dma("sp", lambda e, k=k, s=s: e.dma_start(out=xt[s][:], in_=xe[k * 128:(k + 1) * 128, :]),
                  B_xt[s], (), xsem[s])
            rmsnorm(xt[s][:], [B_xt[s]], 2048, gbc[:], B_gbc, xn[:], B_xn, junk[:], B_junk, 0)
            tau = 64 + 128 * k
            pi = transposes_bf(xn, B_xn, 16, lambda c0, n, tau=tau: hT[:, c0:c0 + n, tau:tau + 128], B_hT, pi)
        P.barrier()

        wq = [0]

        def load_w(src_ap, ncols, kch=16):
            s = wq[0] % 2
            wq[0] += 1
            dst = wbuf[s][:, 0:kch, 0:ncols]
            P.dma("pool", lambda e: e.dma_start(out=dst, in_=src_ap.rearrange("(k p) n -> p k n", p=128)),
                  B_wbuf[s], (), wsem[s])
            return wbuf[s], B_wbuf[s]

        pcnt = [0]

        def next_ps():
            pcnt[0] = (pcnt[0] + 1) % 6
            return pcnt[0]

        OWN0 = 320
        if upto >= 1 and RUN_L0:
            zt = sbt("zt", [128, 1026], F32, o_v)
            gcs = sbt("gcs", [128, 512], F32, o_v + 4128)
            yt = sbt("yt", [128, 1024], F32, o_v + 4128 + 2048)
            B_zt, B_gcs, B_yt = Buf("zt"), Buf("gcs"), Buf("yt")
            for c in range(8):
                s = wq[0] % 2
                wq[0] += 1
                for part, col0 in enumerate((3072, 4096, 5120)):
                    src = w_in_ab[:, col0 + c * 128: col0 + (c + 1) * 128]
                    dstw = wbuf[s][:, :, part * 128:(part + 1) * 128]
                    P.dma("pool", lambda e, dstw=dstw, src=src: e.dma_start(
                        out=dstw, in_=src.rearrange("(k p) n -> p k n", p=128)), B_wbuf[s], (), wsem[s])
                wb, Bw = wbuf[s], B_wbuf[s]
                ph = next_ps()
                halo_rhs = [hT[:, k, OWN0 - 1:OWN0 + 1025:1025] for k in range(16)]
                mm(psum[ph][:, 0:2], [(wb[:, k, 128:256], halo_rhs[k]) for k in range(16)], [Bw, B_hT], [PB[ph]])
                mm(psum[ph][:, 2:4], [(wb[:, k, 256:384], halo_rhs[k]) for k in range(16)], [Bw, B_hT], [PB[ph]])
                P.op("act", lambda e, ph=ph: e.copy(out=gcs[:, 0:2], in_=psum[ph][:, 0:2]), [PB[ph]], [B_gcs])
                P.op("dve", lambda e, ph=ph: e.tensor_tensor(out=zt[:, 0:1026:1025], in0=psum[ph][:, 2:4],
                                                            in1=gcs[:, 0:2], op=ALU.mult),
                     [PB[ph], B_gcs], [B_zt])
                pgb = []
                for tc in range(2):
                    t0 = OWN0 + tc * 512
                    pb_, pc_, ph_ = next_ps(), next_ps(), next_ps()
                    mm(psum[pb_][:], [(wb[:, k, 0:128], hT[:, k, t0:t0 + 512]) for k in range(16)], [Bw, B_hT], [PB[pb_]])
                    mm(psum[pc_][:], [(wb[:, k, 128:256], hT[:, k, t0:t0 + 512]) for k in range(16)], [Bw, B_hT], [PB[pc_]])
                    mm(psum[ph_][:], [(wb[:, k, 256:384], hT[:, k, t0:t0 + 512]) for k in range(16)], [Bw, B_hT], [PB[ph_]])
                    P.op("act", lambda e, pc_=pc_: e.copy(out=gcs[:], in_=psum[pc_][:]), [PB[pc_]], [B_gcs])
                    P.op("dve", lambda e, ph_=ph_, tc=tc: e.tensor_tensor(
                        out=zt[:, 1 + tc * 512:1 + (tc + 1) * 512], in0=psum[ph_][:], in1=gcs[:], op=ALU.mult),
                        [PB[ph_], B_gcs], [B_zt])
                    pgb.append(pb_)
                P.op("dve", lambda e, c=c: e.tensor_scalar(out=yt[:], in0=zt[:, 0:1024], scalar1=cwb[:, c * 3:c * 3 + 1],
                                                          scalar2=None, op0=ALU.mult), [B_zt, B_const], [B_yt])
                P.op("dve", lambda e, c=c: e.scalar_tensor_tensor(out=yt[:], in0=zt[:, 1:1025],
                                                                 scalar=cwb[:, c * 3 + 1:c * 3 + 2], in1=yt[:],
                                                                 op0=ALU.mult, op1=ALU.add), [B_zt, B_yt, B_const], [B_yt])
                P.op("dve", lambda e, c=c: e.scalar_tensor_tensor(out=yt[:], in0=zt[:, 2:1026],
                                                                 scalar=cwb[:, c * 3 + 2:c * 3 + 3], in1=yt[:],
                                                                 op0=ALU.mult, op1=ALU.add), [B_zt, B_yt, B_const], [B_yt])
                for tc in range(2):
                    P.op("dve", lambda e, c=c, tc=tc, pb_=pgb[tc]: e.scalar_tensor_tensor(
                        out=boutT[:, c, tc * 512:(tc + 1) * 512], in0=yt[:, tc * 512:(tc + 1) * 512],
                        scalar=cwb[:, 24 + c:25 + c], in1=psum[pb_][:], op0=ALU.add, op1=ALU.mult),
                        [B_yt, PB[pgb[tc]], B_const], [B_bout])
            P.barrier()

            P.op("pool", lambda e: e.memset(KT[:, :, 0:64], 0.0), [], [B_KT])
            for g in range(2):
                wb, Bw = load_w(w_in_ab[:, g * 512:(g + 1) * 512], 512)
                for hh in range(4):
                    h = g * 4 + hh
                    for tc in range(2):
                        p_ = next_ps()
                        t0 = OWN0 + tc * 512
                        mm(psum[p_][:], [(wb[:, k, hh * 128:(hh + 1) * 128], hT[:, k, t0:t0 + 512]) for k in range(16)],
                           [Bw, B_hT], [PB[p_]])
                        P.op("act", lambda e, p_=p_, h=h, tc=tc: e.copy(out=QT[:, h, tc * 512:(tc + 1) * 512], in_=psum[p_][:]),
                             [PB[p_]], [B_QT])
            for g in range(2):
                wb, Bw = load_w(w_in_ab[:, 1024 + g * 512:1024 + (g + 1) * 512], 512)
                for hh in range(4):
                    h = g * 4 + hh
                    for tc in range(3):
                        p_ = next_ps()
                        t0 = 64 + tc * 512
                        mm(psum[p_][:], [(wb[:, k, hh * 128:(hh + 1) * 128], hT[:, k, t0:t0 + 512]) for k in range(16)],
                           [Bw, B_hT], [PB[p_]])
                        P.op("act", lambda e, p_=p_, h=h, tc=tc: e.copy(
                            out=KT[:, h, 64 + tc * 512:64 + (tc + 1) * 512], in_=psum[p_][:]), [PB[p_]], [B_KT])
            for g in range(2):
                wb, Bw = load_w(w_in_ab[:, 2048 + g * 512:2048 + (g + 1) * 512], 512)
                for idx in range(24):
                    if idx < 12:
                        tau = (2 * (idx + 2) - 3) * 64
                    else:
                        tau = (2 * (idx - 11) + 1 - 3) * 64
                    p_ = next_ps()
                    mm(psum[p_][:], [(hT[:, k, tau:tau + 128], wb[:, k, :]) for k in range(16)], [Bw, B_hT], [PB[p_]])
                    eng = "act" if idx % 2 == 0 else "dve"
                    if eng == "act":
                        P.op("act", lambda e, p_=p_, idx=idx, g=g: e.copy(out=VV[:, idx, g * 512:(g + 1) * 512], in_=psum[p_][:]),
                             [PB[p_]], [B_VV])
                    else:
                        P.op("dve", lambda e, p_=p_, idx=idx, g=g: e.tensor_copy(out=VV[:, idx, g * 512:(g + 1) * 512], in_=psum[p_][:]),
                             [PB[p_]], [B_VV])
        P.barrier()
        P.flush()

        aoutT = sbt("aoutT", [128, 8, 1024], BF16, o_w)
        atab = sbt("atab", [128, 8, 8, 64], F32, o_w + 16384)
        TT = [sbt(f"TT{i}", [128, 512], F32, 0 + i * 2048) for i in range(2)]
        PT = [sbt(f"PT{i}", [128, 512], BF16, 4096 + i * 1024) for i in range(2)]
        rinv = sbt("rinv", [128, 512], F32, 6144)
        emt = sbt("emt", [16, 8, 128], BF16, 8192)
        rqt = sbt("rqt", [16, 16, 64], BF16, 8192 + 2048)
        B_aout, B_atab = Buf("aoutT"), Buf("atab", const=True)
        B_TT, B_PT, B_rinv = [Buf("TT0"), Buf("TT1")], [Buf("PT0"), Buf("PT1")], Buf("rinv")
        if upto >= 2 and RUN_L0:
            cload(atab[:].rearrange("p h m q -> p (h m q)"), atab_d, B_atab)
            cload(emt[:].rearrange("t m p -> t (m p)"), em_d, B_atab)
            cload(rqt[:].rearrange("t i q -> t (i q)"), rq_d, B_atab)
            scale = float(128 ** -0.5)
            it = 0
            for i in range(16):
                po, pr = 4 + (i % 2) * 2, 5 + (i % 2) * 2
                for h in range(8):
                    ps_ = it % 2
                    it += 1

                    def score_fn(e, i=i, h=h, ps_=ps_):
                        ins = None
                        for m in range(8):
                            srow = i + 1 + 2 * m
                            if not (3 <= srow <= 26):
                                srow = 4
                            k0 = (srow - 3) * 64
                            e.matmul(psum[ps_][:, m * 64:(m + 1) * 64], KT[:, h, k0:k0 + 128], QT[:, h, i * 64:(i + 1) * 64],
                                     start=True, stop=False)
                            ins = e.matmul(psum[ps_][:, m * 64:(m + 1) * 64], emt[:, m, :], rqt[:, i, :],
                                           start=False, stop=True)
                        return ins
                    P.op("pe", score_fn, [B_KT, B_QT, B_atab], [PB[ps_]])
                    P.op("dve", lambda e, ps_=ps_, h=h: e.scalar_tensor_tensor(
                        out=TT[ps_][:], in0=psum[ps_][:], scalar=scale, in1=atab[:, h, :, :].rearrange("p m q -> p (m q)"),
                        op0=ALU.mult, op1=ALU.add), [PB[ps_], B_atab], [B_TT[ps_]])
                    P.op("act", lambda e, ps_=ps_: e.activation(out=PT[ps_][:], in_=TT[ps_][:], func=AF.Exp),
                         [B_TT[ps_]], [B_PT[ps_]])

                    def pv_fn(e, i=i, h=h, ps_=ps_, po=po, pr=pr):
                        ins = None
                        for m in range(8):
                            srow = i + 1 + 2 * m
                            if not (3 <= srow <= 26):
                                srow = 4
                            vi = (srow // 2 - 2) if srow % 2 == 0 else (11 + (srow - 1) // 2)
                            e.matmul(psum[po][:, h * 64:(h + 1) * 64], VV[:, vi, h * 128:(h + 1) * 128],
                                     PT[ps_][:, m * 64:(m + 1) * 64], start=(m == 0), stop=(m == 7))
                            ins = e.matmul(psum[pr][:, h * 64:(h + 1) * 64], ones_bf[:], PT[ps_][:, m * 64:(m + 1) * 64],
                                           start=(m == 0), stop=(m == 7))
                        return ins
                    P.op("pe", pv_fn, [B_VV, B_PT[ps_], B_const], [PB[po], PB[pr]])
                P.op("dve", lambda e, pr=pr: e.reciprocal(out=rinv[:], in_=psum[pr][:]), [PB[pr]], [B_rinv])
                P.op("dve", lambda e, po=po, i=i: e.tensor_tensor(
                    out=aoutT[:, :, i * 64:(i + 1) * 64], in0=psum[po][:].rearrange("p (h q) -> p h q", q=64),
                    in1=rinv[:].rearrange("p (h q) -> p h q", q=64), op=ALU.mult), [PB[po], B_rinv], [B_aout])
        P.barrier()
        P.flush()

        wo = [sbt(f"wo{i}", [128, 16, 512], BF16, o_v + i * 16384) for i in range(3)]
        B_wo = [Buf(f"wo{i}") for i in range(3)]
        wosem = [SP_[4], SP_[5], SP_[6]]
        rsem = SP_[7]

        def out_proj(w_dram, lhs_fn, lhs_bufs, wo, wosem):
            for g in range(4):
                s = g % 3
                P.dma("pool", lambda e, g=g, s=s: e.dma_start(
                    out=wo[s][:], in_=w_dram[:, g * 512:(g + 1) * 512].rearrange("(k p) n -> p k n", p=128)),
                    B_wo[s], (), wosem[s])
                for tt in range(8):
                    p_ = next_ps()
                    mm(psum[p_][:], [(lhs_fn(k, tt), wo[s][:, k, :]) for k in range(16)], [B_wo[s]] + lhs_bufs, [PB[p_]])
                    P.op("dve", lambda e, p_=p_, tt=tt, g=g: e.tensor_tensor(
                        out=xres[:, tt, g * 512:(g + 1) * 512], in0=psum[p_][:], in1=xres[:, tt, g * 512:(g + 1) * 512],
                        op=ALU.add), [PB[p_], B_xres[tt]], [B_xres[tt]])

        if not RUN_L0:
            for tt in range(8):
                lt = P.dma("sp", lambda e, tt=tt: e.dma_start(out=xres[:, tt, :], in_=xe[(tt + 2) * 128:(tt + 3) * 128, :]),
                           B_xres[tt], (), rsem)
            for tt in range(8):
                B_xres[tt].w = lt
        if upto >= 3 and RUN_L0:
            for tt in range(8):
                lt = P.dma("sp", lambda e, tt=tt: e.dma_start(out=xres[:, tt, :], in_=xe[(tt + 2) * 128:(tt + 3) * 128, :]),
                           B_xres[tt], (), rsem)
            for tt in range(8):
                B_xres[tt].w = lt
            out_proj(w_out_ab, lambda k, tt: (aoutT[:, k, tt * 128:(tt + 1) * 128] if k < 8
                                              else boutT[:, k - 8, tt * 128:(tt + 1) * 128]), [B_aout, B_bout], wo, wosem)
        P.barrier()
        P.flush()

        def moe(layer):
            o = PH
            h2 = sbt(f"h2_{layer}", [128, 8, 2048], BF16, o); o += 32768
            Gf = sbt(f"Gf{layer}", [128, 8, 64], F32, o); o += 2048
            Ghl = sbt(f"Ghl{layer}", [128, 8, 64, 2], BF16, o); o += 2048
            Msf = sbt(f"Msf{layer}", [128, 8, 64], F32, o); o += 2048
            Msb = sbt(f"Msb{layer}", [128, 8, 64], BF16, o); o += 1024
            posf = sbt(f"posf{layer}", [128, 8, 64], F32, o); o += 2048
            OH = sbt(f"OH{layer}", [128, 8, 2, 128], BF16, o); o += 4096
            OHT = sbt(f"OHT{layer}", [128, 2, 1024], BF16, o); o += 4096
            xeT = sbt(f"xeT{layer}", [128, 16, 2, 128], BF16, o); o += 8192
            HgT = [sbt(f"HgT{layer}{i}", [128, 6, 128], BF16, o + i * 1536) for i in range(2)]; o += 3072
            sgs = sbt(f"sgs{layer}", [128, 768], F32, o); o += 3072
            Yw = sbt(f"Yw{layer}", [128, 2, 2048], BF16, o); o += 8192
            gsl = sbt(f"gsl{layer}", [128, 4], F32, o); o += 32
            o_ring = o
            NS = (PH + PHSZ - o_ring) // 6144
            ring13 = [sbt(f"r13_{layer}_{i}", [128, 4, 768], BF16, o_ring + i * 6144) for i in range(NS)]
            ring2 = [sbt(f"r2_{layer}_{i}", [128, 6, 512], BF16, o_ring + i * 6144) for i in range(NS)]
            B_ring = [Buf(f"ring{i}") for i in range(NS)]
            NS = min(NS, 11)
            ringsem = SP_[0:NS]
            h2f = sbt(f"h2f{layer}", [128, 2048], F32, o_ring)
            h2Tf = sbt(f"h2Tf{layer}", [128, 16, 128], F32, o_ring + 8192)
            wrs = sbt(f"wrs{layer}", [128, 16, 72], F32, o_ring + 16384)
            gb2 = sbt(f"gb2{layer}", [128, 2048], F32, o_ring + 16384 + 4608)
            brs = sbt(f"brs{layer}", [128, 72], F32, o_ring + 16384 + 4608 + 8192)
            lg = sbt(f"lg{layer}", [128, 72], F32, o_ring + 16384 + 4608 + 8192 + 288)
            tmp8 = sbt(f"tmp8{layer}", [128, 64], F32, o_ring + 16384 + 4608 + 8192 + 288 * 2)
            junk2 = sbt(f"junk2{layer}", [128, 2048], BF16, o_ring + 40960)
            B_h2, B_Gf, B_Ghl, B_Msf, B_Msb, B_posf = (Buf("h2"), Buf("Gf"), Buf("Ghl"), Buf("Msf"), Buf("Msb"), Buf("posf"))
            B_OH, B_OHT, B_xeT, B_sgs, B_Yw, B_gsl = Buf("OH"), Buf("OHT"), Buf("xeT"), Buf("sgs"), Buf("Yw"), Buf("gsl")
            B_HgT = [Buf("HgT0"), Buf("HgT1")]
            B_h2f, B_h2Tf, B_rc, B_lg, B_tmp8, B_junk2 = Buf("h2f"), Buf("h2Tf"), Buf("rconst", const=True), Buf("lg"), Buf("tmp8"), Buf("junk2")
            B_gb2 = Buf("gb2")

            cload(wrs[:], wr_d[layer].rearrange("(k p) n -> p k n", p=128), B_rc)
            cload(brs[:], br_d[layer], B_rc)
            cload(gb2[:], gvec[1 + 2 * layer], B_gb2)

            def S(i):
                return small[:, i:i + 1], B_small[i]

            for tt in range(8):
                rmsnorm(xres[:, tt, :], [B_xres[tt]], 2048, gb2[:], B_gb2, h2f[:], B_h2f, junk2[:], B_junk2, 2)
                P.op("act", lambda e, tt=tt: e.copy(out=h2[:, tt, :], in_=h2f[:]), [B_h2f], [B_h2])
                for half in range(4):
                    p_ = next_ps()

                    def tfn(e, half=half, p_=p_):
                        ins = None
                        for c in range(4):
                            cc = half * 4 + c
                            ins = e.transpose(psum[p_][:, c * 128:(c + 1) * 128], h2f[:, cc * 128:(cc + 1) * 128], ident_f[:])
                        return ins
                    P.op("pe", tfn, [B_h2f, B_const], [PB[p_]])
                    P.op("dve", lambda e, half=half, p_=p_: e.tensor_copy(
                        out=h2Tf[:, half * 4:(half + 1) * 4, :], in_=psum[p_][:].rearrange("p (c t) -> p c t", t=128)),
                        [PB[p_]], [B_h2Tf])
                p_ = next_ps()
                mm(psum[p_][:, 0:72], [(h2Tf[:, k, :], wrs[:, k, :]) for k in range(16)], [B_h2Tf, B_rc], [PB[p_]])
                P.op("dve", lambda e, p_=p_: e.tensor_tensor(out=lg[:], in0=psum[p_][:, 0:72], in1=brs[:], op=ALU.add),
                     [PB[p_], B_rc], [B_lg])
                gmax, Bgmax = S(4)
                ngmax, Bng = S(5)
                gsum, Bgs = S(6)
                gprob, Bgp = S(7)
                v1, Bv1 = S(8)
                v2, Bv2 = S(9)
                dd, Bdd = S(10)
                ed, Bed = S(11)
                wa, Bwa = S(12)
                wb_, Bwb = S(13)
                goh, ein, oh1, e2, oh2, ge, gex = (tmp8[:, 0:8], tmp8[:, 8:16], tmp8[:, 16:24], tmp8[:, 24:32],
                                                  tmp8[:, 32:40], tmp8[:, 40:48], tmp8[:, 48:56])
                Bt = B_tmp8
                P.op("dve", lambda e: e.reduce_max(out=gmax, in_=lg[:, 0:8], axis=AX.X), [B_lg], [Bgmax])
                P.op("dve", lambda e: e.tensor_scalar(out=goh, in0=lg[:, 0:8], scalar1=gmax, scalar2=None, op0=ALU.is_equal),
                     [B_lg, Bgmax], [Bt])
                P.op("dve", lambda e: e.tensor_scalar(out=ngmax, in0=gmax, scalar1=-1.0, scalar2=None, op0=ALU.mult),
                     [Bgmax], [Bng])
                P.op("act", lambda e: e.activation(out=gex, in_=lg[:, 0:8], func=AF.Exp, bias=ngmax, scale=1.0, accum_out=gsum),
                     [B_lg, Bng, Bt], [Bt, Bgs])
                P.op("dve", lambda e: e.reciprocal(out=gprob, in_=gsum), [Bgs], [Bgp])
                P.op("dve", lambda e: e.tensor_scalar(out=ein, in0=lg[:, 8:16], scalar1=goh[:, 0:1], scalar2=None, op0=ALU.mult),
                     [B_lg, Bt], [Bt])
                for g in range(1, 8):
                    P.op("dve", lambda e, g=g: e.scalar_tensor_tensor(out=ein, in0=lg[:, 8 + 8 * g:16 + 8 * g], scalar=goh[:, g:g + 1],
                                                                    in1=ein, op0=ALU.mult, op1=ALU.add), [B_lg, Bt], [Bt])
                P.op("dve", lambda e: e.reduce_max(out=v1, in_=ein, axis=AX.X), [Bt], [Bv1])
                P.op("dve", lambda e: e.tensor_scalar(out=oh1, in0=ein, scalar1=v1, scalar2=None, op0=ALU.is_equal), [Bt, Bv1], [Bt])
                P.op("dve", lambda e: e.scalar_tensor_tensor(out=e2, in0=oh1, scalar=-1.0e9, in1=ein, op0=ALU.mult, op1=ALU.add),
                     [Bt], [Bt])
                P.op("dve", lambda e: e.reduce_max(out=v2, in_=e2, axis=AX.X), [Bt], [Bv2])
                P.op("dve", lambda e: e.tensor_scalar(out=oh2, in0=e2, scalar1=v2, scalar2=None, op0=ALU.is_equal), [Bt, Bv2], [Bt])
                P.op("dve", lambda e: e.tensor_tensor(out=dd, in0=v2, in1=v1, op=ALU.subtract), [Bv1, Bv2], [Bdd])
                P.op("act", lambda e: e.activation(out=ed, in_=dd, func=AF.Exp), [Bdd], [Bed])
                P.op("dve", lambda e: e.tensor_scalar(out=wa, in0=ed, scalar1=1.0, scalar2=None, op0=ALU.add), [Bed], [Bwa])
                P.op("dve", lambda e: e.reciprocal(out=wa, in_=wa), [Bwa], [Bwa])
                P.op("dve", lambda e: e.tensor_tensor(out=wb_, in0=ed, in1=wa, op=ALU.mult), [Bed, Bwa], [Bwb])
                P.op("dve", lambda e: e.tensor_tensor(out=wa, in0=wa, in1=gprob, op=ALU.mult), [Bwa, Bgp], [Bwa])
                P.op("dve", lambda e: e.tensor_tensor(out=wb_, in0=wb_, in1=gprob, op=ALU.mult), [Bwb, Bgp], [Bwb])
                P.op("dve", lambda e: e.tensor_scalar(out=ge, in0=oh1, scalar1=wa, scalar2=None, op0=ALU.mult), [Bt, Bwa], [Bt])
                P.op("dve", lambda e: e.scalar_tensor_tensor(out=ge, in0=oh2, scalar=wb_, in1=ge, op0=ALU.mult, op1=ALU.add),
                     [Bt, Bwb], [Bt])
                for g in range(8):
                    P.op("dve", lambda e, g=g, tt=tt: e.tensor_scalar(out=Gf[:, tt, g * 8:(g + 1) * 8], in0=ge, scalar1=goh[:, g:g + 1],
                                                                     scalar2=None, op0=ALU.mult), [Bt], [B_Gf])
                P.op("dve", lambda e, tt=tt: e.tensor_single_scalar(out=Msf[:, tt, :], in_=Gf[:, tt, :], scalar=0.0, op=ALU.is_gt),
                     [B_Gf], [B_Msf])
            P.op("dve", lambda e: e.tensor_copy(out=Msb[:], in_=Msf[:]), [B_Msf], [B_Msb])
            P.op("dve", lambda e: e.tensor_copy(out=Ghl[:, :, :, 0], in_=Gf[:]), [B_Gf], [B_Ghl])
            P.op("dve", lambda e: e.tensor_tensor(out=posf[:], in0=Gf[:], in1=Ghl[:, :, :, 0], op=ALU.subtract), [B_Gf, B_Ghl], [B_posf])
            P.op("dve", lambda e: e.tensor_copy(out=Ghl[:, :, :, 1], in_=posf[:]), [B_posf], [B_Ghl])
            pp = next_ps()

            def posfn(e):
                ins = None
                for i in range(8):
                    ins = e.matmul(psum[pp][:, i * 64:(i + 1) * 64], ustrict[:], Msb[:, i, :], start=True, stop=(i == 0))
                    for i2 in range(i):
                        ins = e.matmul(psum[pp][:, i * 64:(i + 1) * 64], ones_bf[:], Msb[:, i2, :], start=False, stop=(i2 == i - 1))
                return ins
            P.op("pe", posfn, [B_Msb, B_const], [PB[pp]])
            P.op("dve", lambda e: e.scalar_tensor_tensor(out=posf[:].rearrange("p t e -> p (t e)"), in0=psum[pp][:], scalar=1.0,
                                                         in1=Msf[:].rearrange("p t e -> p (t e)"), op0=ALU.add, op1=ALU.mult),
                 [PB[pp], B_Msf, B_Ghl], [B_posf])
            P.op("dve", lambda e: e.tensor_scalar(out=posf[:], in0=posf[:], scalar1=-1.0, scalar2=None, op0=ALU.add), [B_posf], [B_posf])
            P.barrier()

            rq_ = [0]

            def wload(src_ap, is13):
                s = rq_[0] % NS
                rq_[0] += 1
                dst = ring13[s][:] if is13 else ring2[s][:]
                P.dma("pool", lambda e: e.dma_start(out=dst, in_=src_ap.rearrange("(k p) n -> p k n", p=128)),
                      B_ring[s], (), ringsem[s])
                return s

            hgi = [0]
            for eg in range(NEXP // 2):
                for el in range(2):
                    ex = eg * 2 + el
                    for i in range(8):
                        P.op("dve", lambda e, i=i, el=el, ex=ex: e.tensor_scalar(
                            out=OH[:, i, el, :], in0=iota_f[:], scalar1=posf[:, i, ex:ex + 1], scalar2=None, op0=ALU.is_equal),
                            [B_posf, B_const], [B_OH])
                for c in range(16):
                    p_ = next_ps()
                    mm(psum[p_][:, 0:256], [(h2[:, i, c * 128:(c + 1) * 128], OH[:, i, :, :].rearrange("p a s -> p (a s)")) for i in range(8)],
                       [B_h2, B_OH], [PB[p_]])
                    if c % 2 == 0:
                        P.op("act", lambda e, p_=p_, c=c: e.copy(out=xeT[:, c, :, :].rearrange("p a s -> p (a s)"), in_=psum[p_][:, 0:256]),
                             [PB[p_]], [B_xeT])
                    else:
                        P.op("dve", lambda e, p_=p_, c=c: e.tensor_copy(out=xeT[:, c, :, :].rearrange("p a s -> p (a s)"), in_=psum[p_][:, 0:256]),
                             [PB[p_]], [B_xeT])
                for el in range(2):
                    p_ = next_ps()
                    ptb = psum[p_].bitcast(BF16)[:, 0:1024]

                    def ohtfn(e, el=el, ptb=ptb):
                        ins = None
                        for i in range(8):
                            ins = e.transpose(ptb[:, i * 128:(i + 1) * 128], OH[:, i, el, :], ident_bf[:])
                        return ins
                    P.op("pe", ohtfn, [B_OH, B_const], [PB[p_]])
                    P.op("act", lambda e, el=el, ptb=ptb: e.copy(out=OHT[:, el, :], in_=ptb), [PB[p_]], [B_OHT])
                for el in range(2):
                    ex = eg * 2 + el
                    pg = next_ps()
                    mm(psum[pg][:, 0:2], [(OH[:, i, el, :], Ghl[:, i, ex, :]) for i in range(8)], [B_OH, B_Ghl], [PB[pg]])
                    P.op("act", lambda e, pg=pg: e.copy(out=gsl[:, 2:4], in_=psum[pg][:, 0:2]), [PB[pg]], [B_gsl])
                    P.op("dve", lambda e, el=el: e.tensor_tensor(out=gsl[:, el:el + 1], in0=gsl[:, 2:3], in1=gsl[:, 3:4], op=ALU.add),
                         [B_gsl], [B_gsl])
                    ph1a, ph1b, ph3a, ph3b = next_ps(), next_ps(), next_ps(), next_ps()
                    for which, (wd, pa, pb2) in enumerate(((w1_d, ph1a, ph1b), (w3_d, ph3a, ph3b))):
                        for kp in range(4):
                            s = wload(wd[layer][ex, kp * 512:(kp + 1) * 512, :], True)

                            def hfn(e, s=s, kp=kp, pa=pa, pb2=pb2, el=el):
                                ins = None
                                for j in range(6):
                                    dst = psum[pa][:, j * 128:(j + 1) * 128] if j < 4 else psum[pb2][:, (j - 4) * 128:(j - 3) * 128]
                                    for kk in range(4):
                                        k = kp * 4 + kk
                                        ins = e.matmul(dst, ring13[s][:, kk, j * 128:(j + 1) * 128], xeT[:, k, el, :],
                                                       start=(k == 0 and j in (0, 4)), stop=(k == 15), skip_group_check=True)
                                return ins
                            P.op("pe", hfn, [B_ring[s], B_xeT], [PB[pa], PB[pb2]])
                    hb = hgi[0] % 2
                    hgi[0] += 1
                    P.op("act", lambda e, ph1a=ph1a: e.activation(out=sgs[:, 0:512], in_=psum[ph1a][:], func=AF.Silu), [PB[ph1a]], [B_sgs])
                    P.op("act", lambda e, ph1b=ph1b: e.activation(out=sgs[:, 512:768], in_=psum[ph1b][:, 0:256], func=AF.Silu), [PB[ph1b]], [B_sgs])
                    P.op("dve", lambda e, ph3a=ph3a, hb=hb: e.tensor_tensor(
                        out=HgT[hb][:, 0:4, :].rearrange("p j s -> p (j s)"), in0=psum[ph3a][:], in1=sgs[:, 0:512], op=ALU.mult),
                        [PB[ph3a], B_sgs], [B_HgT[hb]])
                    P.op("dve", lambda e, ph3b=ph3b, hb=hb: e.tensor_tensor(
                        out=HgT[hb][:, 4:6, :].rearrange("p j s -> p (j s)"), in0=psum[ph3b][:, 0:256], in1=sgs[:, 512:768], op=ALU.mult),
                        [PB[ph3b], B_sgs], [B_HgT[hb]])
                    for n in range(4):
                        s = wload(w2_d[layer][ex, :, n * 512:(n + 1) * 512], False)
                        py = next_ps()
                        mm(psum[py][:], [(HgT[hb][:, j, :], ring2[s][:, j, :]) for j in range(6)], [B_HgT[hb], B_ring[s]], [PB[py]])
                        P.op("act", lambda e, py=py, el=el, n=n: e.activation(
                            out=Yw[:, el, n * 512:(n + 1) * 512], in_=psum[py][:], func=AF.Copy, scale=gsl[:, el:el + 1]),
                            [PB[py], B_gsl], [B_Yw])
                for tt in range(8):
                    for n in range(4):
                        pu = next_ps()
                        mm(psum[pu][:], [(OHT[:, el, tt * 128:(tt + 1) * 128], Yw[:, el, n * 512:(n + 1) * 512]) for el in range(2)],
                           [B_OHT, B_Yw], [PB[pu]])
                        P.op("dve", lambda e, pu=pu, tt=tt, n=n: e.tensor_tensor(
                            out=xres[:, tt, n * 512:(n + 1) * 512], in0=psum[pu][:], in1=xres[:, tt, n * 512:(n + 1) * 512], op=ALU.add),
                            [PB[pu], B_xres[tt]], [B_xres[tt]])
            P.barrier()
            P.flush()

        if upto >= 4 and RUN_L0:
            moe(0)


        def layer1():
            ag_in = nc.dram_tensor("ag_in", [576, 1024], BF16)
            ag_out = nc.dram_tensor("ag_out", [8 * 576, 1024], BF16)
            B_agin, B_agout = Buf("ag_in"), Buf("ag_out")
            o = PH
            hT1 = sbt("hT1", [128, 16, 1024], BF16, o); o_hT1 = o; o += 32768
            cqnT = sbt("cqnT", [128, 4, 1024], BF16, o); o += 8192
            ugT = sbt("ugT", [128, 8, 1024], BF16, o); o_ug = o; o += 16384
            vn = sbt("vn", [128, 8, 1024], BF16, o); o_vn = o; o += 16384
            ropeC = sbt("ropeC", [64, 1024], F32, o); o += 4096
            ropeS = sbt("ropeS", [64, 1024], F32, o); o += 4096
            protT = sbt("protT", [64, 64], BF16, o); o += 128
            o_t = o
            B_hT1, B_cqnT, B_ugT, B_vn, B_rope = Buf("hT1"), Buf("cqnT"), Buf("ugT"), Buf("vn"), Buf("rope", const=True)
            cload(ropeC[:], rope_d[0], B_rope)
            cload(ropeS[:], rope_d[1], B_rope)
            cload(protT[:], prot_d, B_rope)
            xn1 = sbt("xn1", [128, 2048], BF16, o_t)
            junk1 = sbt("junk1", [128, 2048], BF16, o_t + 4096)
            gbc1 = sbt("gbc1", [128, 2048], F32, o_t + 8192)
            B_xn1, B_junk1, B_gbc1 = Buf("xn1"), Buf("junk1"), Buf("gbc1")
            cload(gbc1[:], gvec[2], B_gbc1)
            pi = 6
            for tt in range(8):
                rmsnorm(xres[:, tt, :], [B_xres[tt]], 2048, gbc1[:], B_gbc1, xn1[:], B_xn1, junk1[:], B_junk1, 0)
                pi = transposes_bf(xn1, B_xn1, 16, lambda c0, n, tt=tt: hT1[:, c0:c0 + n, tt * 128:(tt + 1) * 128], B_hT1, pi)
            P.barrier()
            wb1 = [sbt(f"wb1_{i}", [128, 16, 512], BF16, o_t + i * 16384) for i in range(2)]
            q = o_t + 32768
            qkn = sbt("qkn", [128, 2, 512], F32, q); q += 4096
            sgn = sbt("sgn", [128, 1024], F32, q); q += 4096
            ckvnT = sbt("ckvnT", [128, 4, 1024], BF16, q); q += 8192
            krb = sbt("krb", [64, 1024], BF16, q); q += 2048
            krope = sbt("krope", [64, 1024], BF16, q); q += 2048
            cn = sbt("cn", [128, 512], BF16, q); q += 1024
            g1 = sbt("g1", [128, 1024], F32, q); q += 4096
            g2 = sbt("g2", [128, 1024], F32, q); q += 4096
            assert q <= PH + PHSZ, q
            B_wb1, B_qkn, B_ckvnT, B_krb, B_krope, B_cn, B_g1, B_g2 = ([Buf("wb1_0"), Buf("wb1_1")], Buf("qkn", const=True), Buf("ckvnT"),
                                                                 Buf("krb"), Buf("krope"), Buf("cn"), Buf("g1"), Buf("g2"))
            w1sem = [SP_[0], SP_[1]]
            cload(qkn[:, 0, :], qkn_d[0], B_qkn)
            cload(qkn[:, 1, :], qkn_d[1], B_qkn)
            cload(sgn[:], sgn_d, B_qkn)
            wq1 = [0]

            def load_w1(src_ap, ncols, slot=None):
                s_ = wq1[0] % 2 if slot is None else slot
                wq1[0] += 1
                dst = wb1[s_][:, :, 0:ncols]
                P.dma("pool", lambda e: e.dma_start(out=dst, in_=src_ap.rearrange("(k p) n -> p k n", p=128)),
                      B_wb1[s_], (), w1sem[s_])
                return wb1[s_], B_wb1[s_]

            def gelu_from_psum(ps_ap, ps_buf, out_ap, out_buf, w):
                P.op("act", lambda e: e.activation(out=g1[:, 0:w], in_=ps_ap, func=AF.Square), [ps_buf], [B_g1])
                P.op("dve", lambda e: e.tensor_scalar(out=g1[:, 0:w], in0=g1[:, 0:w], scalar1=0.044715, scalar2=1.0,
                                                      op0=ALU.mult, op1=ALU.add), [B_g1], [B_g1])
                P.op("dve", lambda e: e.tensor_tensor(out=g1[:, 0:w], in0=g1[:, 0:w], in1=ps_ap, op=ALU.mult), [B_g1, ps_buf], [B_g1])
                P.op("act", lambda e: e.activation(out=g1[:, 0:w], in_=g1[:, 0:w], func=AF.Sigmoid, scale=1.5957691216), [B_g1], [B_g1])
                P.op("dve", lambda e: e.tensor_tensor(out=out_ap, in0=g1[:, 0:w], in1=ps_ap, op=ALU.mult), [B_g1, ps_buf], [out_buf])

            for which, (dstT, B_dstT) in enumerate(((cqnT, B_cqnT), (ckvnT, B_ckvnT))):
                wb, Bw = load_w1(w_in_cd[:, which * 512:(which + 1) * 512], 512)
                for tt in range(8):
                    p_ = next_ps()
                    mm(psum[p_][:], [(hT1[:, k, tt * 128:(tt + 1) * 128], wb[:, k, :]) for k in range(16)], [Bw, B_hT1], [PB[p_]])
                    rmsnorm(psum[p_][:], [PB[p_]], 512, qkn[:, which, :], B_qkn, cn[:], B_cn, g1[:, 0:512], B_g1, 2)
                    pi = transposes_bf(cn, B_cn, 4, lambda c0, n, tt=tt, dstT=dstT: dstT[:, c0:c0 + n, tt * 128:(tt + 1) * 128], B_dstT, pi)
            SK = ""
            agsem = SP_[2]
            for c in (range(4) if "A" not in SK else ()):
                lt = P.dma("sp", lambda e, c=c: e.dma_start(out=ag_in.ap()[c * 128:(c + 1) * 128, :], in_=ckvnT[:, c, :]),
                           B_agin, [B_ckvnT], agsem)
            wb, Bw = load_w1(w_in_cd[:, 1024:1088], 64)
            for tc in (range(2) if "K" not in SK else ()):
                pa, pb_ = next_ps(), next_ps()
                mm(psum[pa][0:64, :], [(wb[:, k, 0:64], hT1[:, k, tc * 512:(tc + 1) * 512]) for k in range(16)], [Bw, B_hT1], [PB[pa]])
                P.op("act", lambda e, pa=pa, tc=tc: e.copy(out=krb[:, tc * 512:(tc + 1) * 512], in_=psum[pa][0:64, :]), [PB[pa]], [B_krb])
                if "1" in SK:
                    continue
                P.op("dve", lambda e, pa=pa, tc=tc: e.tensor_tensor(out=g1[0:64, 0:512], in0=psum[pa][0:64, :], in1=ropeC[:, tc * 512:(tc + 1) * 512],
                                                                  op=ALU.mult), [PB[pa], B_rope], [B_g1])
                if "2" in SK:
                    continue
                mm(psum[pb_][0:64, :], [(protT[:], krb[:, tc * 512:(tc + 1) * 512])], [B_krb, B_rope], [PB[pb_]])
                if "3" in SK:
                    continue
                P.op("dve", lambda e, pb_=pb_, tc=tc: e.tensor_tensor(out=g2[0:64, 0:512], in0=psum[pb_][0:64, :], in1=ropeS[:, tc * 512:(tc + 1) * 512],
                                                                   op=ALU.mult), [PB[pb_], B_rope], [B_g2])
                P.op("dve", lambda e, tc=tc: e.tensor_tensor(out=krope[:, tc * 512:(tc + 1) * 512], in0=g1[0:64, 0:512], in1=g2[0:64, 0:512], op=ALU.add),
                     [B_g1, B_g2], [B_krope])
            if "A" not in SK:
                lt = P.dma("sp", lambda e: e.dma_start(out=ag_in.ap()[512:576, :], in_=krope[:]), B_agin, [B_krope], agsem)
                B_agin.w = lt
            P.op("pool", lambda e: e.collective_compute("AllGather", ALU.bypass, replica_groups=[list(range(8))],
                                                        ins=[ag_in.ap().opt()], outs=[ag_out.ap().opt()]),
                 [B_agin], [B_agout])
            for g in (range(2) if "U" not in SK else ()):
                wb, Bw = load_w1(w_in_cd[:, 1088 + g * 512:1088 + (g + 1) * 512], 512)
                for cc in range(4):
                    for tc in range(2):
                        p_ = next_ps()
                        mm(psum[p_][:], [(wb[:, k, cc * 128:(cc + 1) * 128], hT1[:, k, tc * 512:(tc + 1) * 512]) for k in range(16)],
                           [Bw, B_hT1], [PB[p_]])
                        gelu_from_psum(psum[p_][:], PB[p_], ugT[:, g * 4 + cc, tc * 512:(tc + 1) * 512], B_ugT, 512)
            wv = [load_w1(w_in_cd[:, 2112 + g * 512:2112 + (g + 1) * 512], 512, slot=g) for g in range(2)]
            for tt in (range(8) if "V" not in SK else ()):
                for g in range(2):
                    p_ = next_ps()
                    mm(psum[p_][:], [(hT1[:, k, tt * 128:(tt + 1) * 128], wv[g][0][:, k, :]) for k in range(16)], [wv[g][1], B_hT1], [PB[p_]])
                    gelu_from_psum(psum[p_][:], PB[p_], g2[:, g * 512:(g + 1) * 512], B_g2, 512)
                rmsnorm(g2[:], [B_g2], 1024, sgn[:], B_qkn, vn[:, tt, :], B_vn, g1[:], B_g1, 2)
            P.barrier()
            P.flush()
            if upto < 6:
                return
            doutT = sbt("doutT", [128, 8, 1024], BF16, o_t)
            wsT = sbt("wsT", [128, 8, 128], BF16, o_t + 16384)
            bsT = sbt("bsT", [128, 1024], F32, o_t + 16384 + 2048)
            sgt = sbt("sgt", [128, 1024], F32, o_t + 16384 + 2048 + 4096)
            B_dout, B_ws, B_sgt = Buf("doutT"), Buf("wsT", const=True), Buf("sgt")
            cload(wsT[:].rearrange("q g p -> q (g p)"), wsT_d, B_ws, eng="pool")
            cload(bsT[:], bsT_d, B_ws)
            for n in range(8):
                for half in range(2):
                    p_ = next_ps()

                    def sgfn(e, n=n, half=half, p_=p_):
                        ins = None
                        for gg in range(4):
                            g = half * 4 + gg
                            ins = e.matmul(psum[p_][:, gg * 128:(gg + 1) * 128], vn[:, n, g * 128:(g + 1) * 128], wsT[:, g, :],
                                           start=True, stop=True)
                        return ins
                    P.op("pe", sgfn, [B_vn, B_ws], [PB[p_]])
                    P.op("dve", lambda e, p_=p_, half=half: e.tensor_tensor(out=sgt[:, half * 512:(half + 1) * 512], in0=psum[p_][:],
                                                                          in1=bsT[:, half * 512:(half + 1) * 512], op=ALU.add),
                         [PB[p_], B_ws], [B_sgt])
                    P.op("dve", lambda e, n=n, half=half: e.tensor_tensor(
                        out=doutT[:, half * 4:(half + 1) * 4, n * 128:(n + 1) * 128],
                        in0=sgt[:, half * 512:(half + 1) * 512].rearrange("p (g t) -> p g t", t=128),
                        in1=ugT[:, half * 4:(half + 1) * 4, n * 128:(n + 1) * 128], op=ALU.mult), [B_sgt, B_ugT], [B_dout])
            P.barrier()
            P.flush()
            ckv_all = sbt("ckv_all", [128, 4, 4096], BF16, o_hT1)
            ckv_b = sbt("ckv_b", [128, 4, 4096], BF16, o_ug)
            qnT = sbt("qnT", [128, 1024], BF16, o_ug + 8192)
            qrT = sbt("qrT", [64, 1024], BF16, o_ug + 8192 + 2048)
            qrb = sbt("qrb", [64, 1024], BF16, o_ug + 8192 + 4096)
            KnT = sbt("KnT", [128, 4096], BF16, o_vn)
            Vh = sbt("Vh", [128, 32, 128], BF16, o_vn + 8192)
            q = o_t + 16384
            coutT = sbt("coutT", [128, 8, 1024], BF16, q); q += 16384
            wuq_h = [sbt(f"wuq_h{i}", [128, 4, 192], BF16, q + i * 1536) for i in range(2)]; q += 3072
            wukv_h = [sbt(f"wukv_h{i}", [128, 4, 256], BF16, q + i * 2048) for i in range(2)]; q += 4096
            m1 = sbt("m1", [64, 512], F32, q); q += 2048
            m2 = sbt("m2", [64, 512], F32, q); q += 2048
            PT2 = [sbt(f"PT2_{i}", [128, 512], BF16, q + i * 1024) for i in range(2)]; q += 2048
            rinv2 = sbt("rinv2", [128, 512], F32, q); q += 2048
            krope_all = sbt("krope_all", [64, 4096], BF16, q); q += 8192
            krope_b = sbt("krope_b", [64, 4096], BF16, o_t + 16384)
            bmt = sbt("bmt", [128, 2], F32, CO + 1792 + 160)
            assert q <= PH + PHSZ, q
            B_ckv, B_kra, B_qnT, B_qrT, B_qrb, B_KnT, B_Vh, B_cout = (Buf("ckv_all"), Buf("krope_all"), Buf("qnT"), Buf("qrT"), Buf("qrb"),
                                                                 Buf("KnT"), Buf("Vh"), Buf("coutT"))
            B_wuq, B_wukv, B_m1, B_m2, B_PT2, B_rinv2 = ([Buf("wuq0"), Buf("wuq1")], [Buf("wukv0"), Buf("wukv1")], Buf("m1"), Buf("m2"),
                                                       [Buf("PT2_0"), Buf("PT2_1")], Buf("rinv2"))
            hsem = [SP_[4], SP_[5], SP_[6], SP_[7]]
            lsem = SP_[3]
            B_ckvb, B_krb2, B_bm = Buf("ckv_b"), Buf("krope_b"), Buf("bm", const=True)
            cload(bmt[:], bm_d, B_bm)
            for r in range(4):
                for (cand, kc_, r0) in ((ckv_all, krope_all, r), (ckv_b, krope_b, 4 + r)):
                    lt1 = P.dma("sp", lambda e, r=r, cand=cand, r0=r0: e.dma_start(
                        out=cand[:, :, r * 1024:(r + 1) * 1024],
                        in_=ag_out.ap()[r0 * 576:r0 * 576 + 512, :].rearrange("(c p) t -> p c t", p=128)),
                        B_ckv, [B_agout], lsem)
                    lt2 = P.dma("sp", lambda e, r=r, kc_=kc_, r0=r0: e.dma_start(
                        out=kc_[:, r * 1024:(r + 1) * 1024], in_=ag_out.ap()[r0 * 576 + 512:r0 * 576 + 576, :]),
                        B_kra, [B_agout], lsem)
            B_ckv.w = lt2
            B_kra.w = lt2
            for c in range(4):
                P.op("dve", lambda e, c=c: e.tensor_scalar(out=ckv_all[:, c, :], in0=ckv_all[:, c, :], scalar1=bmt[:, 0:1], scalar2=None,
                                                          op0=ALU.mult), [B_ckv, B_bm], [B_ckv])
                P.op("dve", lambda e, c=c: e.scalar_tensor_tensor(out=ckv_all[:, c, :], in0=ckv_b[:, c, :], scalar=bmt[:, 1:2], in1=ckv_all[:, c, :],
                                                                 op0=ALU.mult, op1=ALU.add), [B_ckv, B_bm], [B_ckv])
            P.op("dve", lambda e: e.tensor_scalar(out=krope_all[:], in0=krope_all[:], scalar1=bmt[0:64, 0:1], scalar2=None, op0=ALU.mult),
                 [B_kra, B_bm], [B_kra])
            P.op("dve", lambda e: e.scalar_tensor_tensor(out=krope_all[:], in0=krope_b[:], scalar=bmt[0:64, 1:2], in1=krope_all[:],
                                                         op0=ALU.mult, op1=ALU.add), [B_kra, B_bm], [B_kra])
            P.barrier()
            SC = float(192 ** -0.5)
            p23 = [0]

            def ps23():
                p23[0] = (p23[0] + 1) % 2
                return 2 + p23[0]

            it = 0
            for h in range(8):
                hs = h % 2
                P.dma("pool", lambda e, h=h, hs=hs: e.dma_start(out=wuq_h[hs][:], in_=w_uq[:, h * 192:(h + 1) * 192].rearrange("(k p) n -> p k n", p=128)),
                      B_wuq[hs], (), hsem[hs])
                P.dma("pool", lambda e, h=h, hs=hs: e.dma_start(out=wukv_h[hs][:], in_=w_ukv[:, h * 256:(h + 1) * 256].rearrange("(k p) n -> p k n", p=128)),
                      B_wukv[hs], (), hsem[2 + hs])
                for kc in range(8):
                    p_ = ps23()
                    mm(psum[p_][:], [(wukv_h[hs][:, k, 0:128], ckv_all[:, k, kc * 512:(kc + 1) * 512]) for k in range(4)], [B_wukv[hs], B_ckv], [PB[p_]])
                    if kc % 2 == 0:
                        P.op("act", lambda e, p_=p_, kc=kc: e.copy(out=KnT[:, kc * 512:(kc + 1) * 512], in_=psum[p_][:]), [PB[p_]], [B_KnT])
                    else:
                        P.op("dve", lambda e, p_=p_, kc=kc: e.tensor_copy(out=KnT[:, kc * 512:(kc + 1) * 512], in_=psum[p_][:]), [PB[p_]], [B_KnT])
                for k4 in range(8):
                    p_ = ps23()

                    def vfn(e, k4=k4, p_=p_, hs=hs):
                        ins = None
                        for jj in range(4):
                            kt = k4 * 4 + jj
                            for k in range(4):
                                ins = e.matmul(psum[p_][:, jj * 128:(jj + 1) * 128], ckv_all[:, k, kt * 128:(kt + 1) * 128], wukv_h[hs][:, k, 128:256],
                                               start=(k == 0), stop=(k == 3))
                        return ins
                    P.op("pe", vfn, [B_wukv[hs], B_ckv], [PB[p_]])
                    if k4 % 2 == 0:
                        P.op("act", lambda e, p_=p_, k4=k4: e.copy(out=Vh[:, k4 * 4:(k4 + 1) * 4, :].rearrange("p a d -> p (a d)"), in_=psum[p_][:]), [PB[p_]], [B_Vh])
                    else:
                        P.op("dve", lambda e, p_=p_, k4=k4: e.tensor_copy(out=Vh[:, k4 * 4:(k4 + 1) * 4, :].rearrange("p a d -> p (a d)"), in_=psum[p_][:]), [PB[p_]], [B_Vh])
                for tc in range(2):
                    p_ = ps23()
                    mm(psum[p_][:], [(wuq_h[hs][:, k, 0:128], cqnT[:, k, tc * 512:(tc + 1) * 512]) for k in range(4)], [B_wuq[hs], B_cqnT], [PB[p_]])
                    P.op("act", lambda e, p_=p_, tc=tc: e.activation(out=qnT[:, tc * 512:(tc + 1) * 512], in_=psum[p_][:], func=AF.Copy, scale=SC),
                         [PB[p_]], [B_qnT])
                    pa, pb_ = ps23(), ps23()
                    mm(psum[pa][0:64, :], [(wuq_h[hs][:, k, 128:192], cqnT[:, k, tc * 512:(tc + 1) * 512]) for k in range(4)], [B_wuq[hs], B_cqnT], [PB[pa]])
                    P.op("act", lambda e, pa=pa, tc=tc: e.copy(out=qrb[:, tc * 512:(tc + 1) * 512], in_=psum[pa][0:64, :]), [PB[pa]], [B_qrb])
                    P.op("dve", lambda e, pa=pa, tc=tc: e.tensor_tensor(out=m1[:], in0=psum[pa][0:64, :], in1=ropeC[:, tc * 512:(tc + 1) * 512], op=ALU.mult),
                         [PB[pa], B_rope], [B_m1])
                    mm(psum[pb_][0:64, :], [(protT[:], qrb[:, tc * 512:(tc + 1) * 512])], [B_qrb, B_rope], [PB[pb_]])
                    P.op("dve", lambda e, pb_=pb_, tc=tc: e.tensor_tensor(out=m2[:], in0=psum[pb_][0:64, :], in1=ropeS[:, tc * 512:(tc + 1) * 512], op=ALU.mult),
                         [PB[pb_], B_rope], [B_m2])
                    P.op("dve", lambda e: e.tensor_tensor(out=m1[:], in0=m1[:], in1=m2[:], op=ALU.add), [B_m1, B_m2], [B_m1])
                    P.op("dve", lambda e, tc=tc: e.tensor_scalar(out=qrT[:, tc * 512:(tc + 1) * 512], in0=m1[:], scalar1=SC, scalar2=None, op0=ALU.mult),
                         [B_m1], [B_qrT])
                for tc in range(2):
                    po, pr = 4 + (tc % 2) * 2, 5 + (tc % 2) * 2
                    for kt in range(32):
                        ps_ = it % 2
                        it += 1

                        def sfn(e, kt=kt, tc=tc, ps_=ps_):
                            e.matmul(psum[ps_][:], KnT[:, kt * 128:(kt + 1) * 128], qnT[:, tc * 512:(tc + 1) * 512], start=True, stop=False)
                            return e.matmul(psum[ps_][:], krope_all[:, kt * 128:(kt + 1) * 128], qrT[:, tc * 512:(tc + 1) * 512], start=False, stop=True)
                        P.op("pe", sfn, [B_KnT, B_qnT, B_kra, B_qrT], [PB[ps_]])
                        P.op("act", lambda e, ps_=ps_: e.activation(out=PT2[ps_][:], in_=psum[ps_][:], func=AF.Exp), [PB[ps_]], [B_PT2[ps_]])

                        def pvfn(e, kt=kt, ps_=ps_, po=po, pr=pr):
                            e.matmul(psum[po][:], Vh[:, kt, :], PT2[ps_][:], start=(kt == 0), stop=(kt == 31))
                            return e.matmul(psum[pr][:], ones_bf[:], PT2[ps_][:], start=(kt == 0), stop=(kt == 31))
                        P.op("pe", pvfn, [B_Vh, B_PT2[ps_], B_const], [PB[po], PB[pr]])
                    P.op("dve", lambda e, pr=pr: e.reciprocal(out=rinv2[:], in_=psum[pr][:]), [PB[pr]], [B_rinv2])
                    P.op("dve", lambda e, po=po, h=h, tc=tc: e.tensor_tensor(out=coutT[:, h, tc * 512:(tc + 1) * 512], in0=psum[po][:], in1=rinv2[:], op=ALU.mult),
                         [PB[po], B_rinv2], [B_cout])
            P.barrier()
            P.flush()
            wo1 = [sbt(f"wo1_{i}", [128, 16, 512], BF16, PH + i * 16384) for i in range(3)]
            out_proj(w_out_cd, lambda k, tt: (coutT[:, k, tt * 128:(tt + 1) * 128] if k < 8 else doutT[:, k - 8, tt * 128:(tt + 1) * 128]),
                     [B_cout, B_dout], wo1, [SP_[8], SP_[9], SP_[10]])
            P.barrier()
            P.flush()

        if upto >= 5 and not skip_l1:
            layer1()
        if upto >= 7:
            moe(1)

        def store_out(normed):
            gf = sbt("gfin", [128, 2048], F32, PH)
            ot = [sbt(f"ot{i}", [128, 2048], F32, PH + 8192 + i * 8192) for i in range(2)]
            jk = sbt("jkf", [128, 2048], BF16, PH + 8192 * 3)
            B_gf, B_ot, B_jk = Buf("gfin"), [Buf("ot0"), Buf("ot1")], Buf("jkf")
            osem = [SP_[0], SP_[1]]
            if normed:
                cload(gf[:], gvec[4], B_gf)
            for tt in range(8):
                s = tt % 2
                if normed:
                    rmsnorm(xres[:, tt, :], [B_xres[tt]], 2048, gf[:], B_gf, ot[s][:], B_ot[s], jk[:], B_jk, 20)
                    P.dma("sp", lambda e, tt=tt, s=s: e.dma_start(out=y_d[tt * 128:(tt + 1) * 128, :], in_=ot[s][:]),
                          Buf("ydram"), [B_ot[s]], osem[s])
                else:
                    P.dma("sp", lambda e, tt=tt: e.dma_start(out=y_d[tt * 128:(tt + 1) * 128, :], in_=xres[:, tt, :]),
                          Buf("ydram"), [B_xres[tt]], osem[s])

        if upto < 99:
            store_out(False)
        else:
            store_out(True)
        P.flush()
        P.final_wait("sp")
        P.final_wait("pool")
    nc._declared = declared
    nc._cnt = dict(P.cnt)
    nc._nins = P.n_ins
    return nc


def make_in_maps(inp, cores=range(8)):
    x = np.asarray(inp["x"], np.float32)
    consts = _consts()
    rep = lambda v, n=128: np.ascontiguousarray(np.broadcast_to(np.asarray(v, np.float32)[None, :], (n, v.shape[-1])))
    gvec = np.stack([rep(inp["norm_mix"][0]), rep(inp["norm_ffn"][0]), rep(inp["norm_mix"][1]),
                     rep(inp["norm_ffn"][1]), rep(inp["norm_final"])])
    atab = _na_tables(np.asarray(inp["na_rpb"][0], np.float32))
    cw = np.asarray(inp["conv_w"][0], np.float32)
    cbias = np.asarray(inp["conv_b"][0], np.float32)
    cwb = np.zeros((128, 32), np.float32)
    cwb[:, 0:24] = cw.reshape(3, 8, 128).transpose(2, 1, 0).reshape(128, 24)
    cwb[:, 24:32] = cbias.reshape(8, 128).T
    wr = np.ascontiguousarray(np.concatenate([inp["router_group_w"], inp["router_expert_w"]], axis=-1), dtype=np.float32)
    br = np.concatenate([inp["router_group_b"], inp["router_expert_b"]], axis=-1).astype(np.float32)
    br = np.ascontiguousarray(np.broadcast_to(br[:, None, :], (2, 128, 72)))
    qkn = np.stack([rep(inp["q_norm"][0]), rep(inp["kv_norm"][0])])
    sgn = rep(inp["sg_norm"][0])
    wsT = np.ascontiguousarray(np.asarray(inp["sg_w"][0], np.float32).transpose(2, 0, 1).reshape(128, 1024))
    bsT = np.ascontiguousarray(np.broadcast_to(np.asarray(inp["sg_b"][0], np.float32).reshape(1, 1024), (128, 1024)))
    shared = dict(gvec=gvec, w_in_ab=np.asarray(inp["w_in_ab"][0], np.float32), w_out_ab=np.asarray(inp["w_out_ab"][0], np.float32),
                  atab=atab, em=consts["em"], cwb=cwb, ident_bf=consts["ident_bf"], ident_f=consts["ident_f"],
                  ones_bf=consts["ones_bf"], ustrict=consts["ustrict"], iota_f=consts["iota_f"], wr=wr, br=br,
                  w1_0=np.asarray(inp["w1"][0], np.float32), w3_0=np.asarray(inp["w3"][0], np.float32), w2_0=np.asarray(inp["w2"][0], np.float32),
                  w1_1=np.asarray(inp["w1"][-1], np.float32), w3_1=np.asarray(inp["w3"][-1], np.float32), w2_1=np.asarray(inp["w2"][-1], np.float32),
                  w_in_cd=np.asarray(inp["w_in_cd"][0], np.float32), qkn=qkn, w_uq=np.asarray(inp["w_uq"][0], np.float32),
                  w_ukv=np.asarray(inp["w_ukv"][0], np.float32), sgn=sgn, wsT=wsT, bsT=bsT,
                  w_out_cd=np.asarray(inp["w_out_cd"][0], np.float32), prot=consts["prot"])
    maps = []
    for c in cores:
        b, j = c // 4, c % 4
        xeh = np.zeros((24, 64, 2048), np.float32)
        for e in range(4, 28):
            g = 16 * j - 8 + e
            if 0 <= g < 64:
                xeh[e - 4] = x[b, g * 64:(g + 1) * 64, :]
        m = dict(shared)
        m["xe"] = xeh.reshape(1536, 2048)
        m["rq"] = _row_mask(j)
        m["ropecs"] = _rope_tables(j)
        bm = np.zeros((128, 2), np.float32)
        bm[:, b] = 1.0
        m["bm"] = bm
        maps.append(m)
    return maps


_NC_CACHE = {}
MODE = "fused"


def _gather(res, key="y"):
    out = np.zeros((2, 4096, 2048), np.float32)
    for c in range(8):
        b, j = c // 4, c % 4
        out[b, j * 1024:(j + 1) * 1024, :] = res.results[c][key]
    return out


def kernel(**inputs):
    if MODE == "fused":
        if "nc" not in _NC_CACHE:
            _NC_CACHE["nc"] = build_program()
        nc = _NC_CACHE["nc"]
        maps = make_in_maps(inputs)
        maps = [{k: m[k] for k in nc._declared} for m in maps]
        return _gather(run_bass_kernel_spmd(nc, maps, core_ids=list(range(8))))
    if "a" not in _NC_CACHE:
        _NC_CACHE["a"] = build_program(upto=4)
        _NC_CACHE["b"] = build_program(upto=6, skip_l0=True)
        _NC_CACHE["c"] = build_program(upto=99, skip_l0=True, skip_l1=True)
    x = None
    for key in ("a", "b", "c"):
        ncx = _NC_CACHE[key]
        inp = dict(inputs)
        if x is not None:
            inp["x"] = x
        maps = make_in_maps(inp)
        x = _gather(run_bass_kernel_spmd(ncx, [{k: m[k] for k in ncx._declared} for m in maps], core_ids=list(range(8))))
    return x
```
